# Optimizing a Trainium2 kernel written in Bass

```python
import jax, jax.numpy as jnp
from jax import lax
import numpy as np

D_MODEL = 1024
BATCH = 16
SEQ = 2048
DEPTH = 1

GRID_W = 64
N_HEADS = 8
N_KV_HEADS = 2
HEAD_DIM = 64
ATTN_WIDTH = N_HEADS * HEAD_DIM
KV_WIDTH = N_KV_HEADS * HEAD_DIM
CONV_WIDTH = 512
CONV_K = 3
ROPE_THETA = 10000.0
Q_BLOCK = 128
N_EXPERTS = 256
TOP_K = 8
N_GROUPS = 8
TOPK_GROUPS = 4
EXPERT_FF = 256
SHARED_FF = 256
ROUTED_SCALE = 2.5
EXPERT_BLOCK = 128
EPS = 1e-6
N_MOD = 6
_SIZES = (ATTN_WIDTH, KV_WIDTH, KV_WIDTH, CONV_WIDTH, CONV_WIDTH, CONV_WIDTH, D_MODEL, D_MODEL)
IN_COLS = sum(_SIZES)
SPLITS = tuple(int(v) for v in np.cumsum(_SIZES)[:-1])

kernel_name = "hybrid_gqa_shortconv_moe_adaln_block"


def rms_norm(x, g):
    xf = x.astype(jnp.float32)
    y = xf * lax.rsqrt(jnp.mean(xf * xf, axis=-1, keepdims=True) + EPS)
    return (y * g.astype(jnp.float32)).astype(x.dtype)


def axial_angles(seq_len):
    rows = seq_len // GRID_W
    row = jnp.repeat(jnp.arange(rows, dtype=jnp.int32), GRID_W).astype(jnp.float32)
    col = jnp.tile(jnp.arange(GRID_W, dtype=jnp.int32), rows).astype(jnp.float32)
    axis_dim = HEAD_DIM // 2
    inv_freq = ROPE_THETA ** (-jnp.arange(0, axis_dim, 2, dtype=jnp.float32) / axis_dim)
    return row[:, None] * inv_freq[None, :], col[:, None] * inv_freq[None, :]


def rope_rotate(x, ang):
    x1, x2 = jnp.split(x, 2, axis=-1)
    cos = jnp.cos(ang).astype(x.dtype)
    sin = jnp.sin(ang).astype(x.dtype)
    return jnp.concatenate([x1 * cos - x2 * sin, x2 * cos + x1 * sin], axis=-1)


def axial_rope(x, ang_r, ang_c):
    xr, xc = jnp.split(x, 2, axis=-1)
    return jnp.concatenate([rope_rotate(xr, ang_r), rope_rotate(xc, ang_c)], axis=-1)


def gqa_attention(q, k, v):
    b, h, s, dh = q.shape
    g = k.shape[1]
    rep = h // g
    nb = s // Q_BLOCK
    qb = q.reshape(b, g, rep, nb, Q_BLOCK, dh).transpose(3, 0, 1, 2, 4, 5)
    scale = dh ** -0.5

    def block(qi):
        sc = jnp.einsum('bgrqd,bgkd->bgrqk', qi, k).astype(jnp.float32) * scale
        p = jax.nn.softmax(sc, axis=-1).astype(v.dtype)
        return jnp.einsum('bgrqk,bgkd->bgrqd', p, v)

    o = lax.map(block, qb)
    return o.transpose(1, 0, 4, 2, 3, 5).reshape(b, s, h * dh)


def short_conv(u, w):
    ch = u.shape[-1]
    return lax.conv_general_dilated(
        u, w[:, None, :].astype(u.dtype), window_strides=(1,),
        padding=((CONV_K // 2, CONV_K // 2),),
        dimension_numbers=('NWC', 'WIO', 'NWC'), feature_group_count=ch)


def token_mixer(h, w_in, q_norm_g, k_norm_g, conv_w, w_attn_o, w_conv_o, w_out):
    b, s, _ = h.shape
    proj = h @ w_in
    q, k, v, cb, cc, cx, ga, gc = jnp.split(proj, SPLITS, axis=-1)
    q = q.reshape(b, s, N_HEADS, HEAD_DIM).transpose(0, 2, 1, 3)
    k = k.reshape(b, s, N_KV_HEADS, HEAD_DIM).transpose(0, 2, 1, 3)
    v = v.reshape(b, s, N_KV_HEADS, HEAD_DIM).transpose(0, 2, 1, 3)
    ang_r, ang_c = axial_angles(s)
    q = axial_rope(rms_norm(q, q_norm_g), ang_r, ang_c)
    k = axial_rope(rms_norm(k, k_norm_g), ang_r, ang_c)
    attn_d = gqa_attention(q, k, v) @ w_attn_o
    conv_d = (cb * short_conv(cc * cx, conv_w)) @ w_conv_o
    merged = jax.nn.sigmoid(ga) * attn_d + jax.nn.sigmoid(gc) * conv_d
    return merged @ w_out


def route(hf, w_router, router_bias):
    t = hf.shape[0]
    scores = jax.nn.sigmoid((hf @ w_router).astype(jnp.float32))
    biased = scores + router_bias.astype(jnp.float32)
    grp = biased.reshape(t, N_GROUPS, N_EXPERTS // N_GROUPS)
    grp_score = jnp.sum(lax.top_k(grp, 2)[0], axis=-1)
    _, grp_idx = lax.top_k(grp_score, TOPK_GROUPS)
    grp_mask = jnp.any(grp_idx[..., None] == jnp.arange(N_GROUPS), axis=-2)
    expert_mask = jnp.repeat(grp_mask, N_EXPERTS // N_GROUPS, axis=-1)
    _, idx = lax.top_k(jnp.where(expert_mask, biased, -jnp.inf), TOP_K)
    w = jnp.take_along_axis(scores, idx, axis=-1)
    w = w / jnp.sum(w, axis=-1, keepdims=True) * ROUTED_SCALE
    return idx, w


def routed_experts(hf, idx, w, w_gate_e, w_up_e, w_down_e):
    t, d = hf.shape
    n_assign = t * TOP_K
    e_flat = idx.reshape(-1)
    tok_flat = jnp.repeat(jnp.arange(t, dtype=jnp.int32), TOP_K)
    w_flat = w.reshape(-1)
    order = jnp.argsort(e_flat)
    e_sorted = e_flat[order]
    counts = jnp.bincount(e_flat, length=N_EXPERTS)
    starts = jnp.cumsum(counts) - counts
    padded = (counts + EXPERT_BLOCK - 1) // EXPERT_BLOCK * EXPERT_BLOCK
    pends = jnp.cumsum(padded)
    pstarts = pends - padded
    dest = pstarts[e_sorted] + (jnp.arange(n_assign) - starts[e_sorted])
    n_blocks = -(-(n_assign + N_EXPERTS * (EXPERT_BLOCK - 1)) // EXPERT_BLOCK)
    cap = n_blocks * EXPERT_BLOCK
    buf_tok = jnp.zeros((cap,), jnp.int32).at[dest].set(tok_flat[order])
    buf_w = jnp.zeros((cap,), w.dtype).at[dest].set(w_flat[order])
    block_e = jnp.searchsorted(pends, jnp.arange(n_blocks) * EXPERT_BLOCK, side='right')
    block_e = jnp.minimum(block_e, N_EXPERTS - 1)

    def block(args):
        tok, wb, e = args
        xb = hf[tok]
        hid = jax.nn.silu(xb @ w_gate_e[e]) * (xb @ w_up_e[e])
        return (hid @ w_down_e[e]) * wb[:, None].astype(xb.dtype)

    out = lax.map(block, (buf_tok.reshape(n_blocks, EXPERT_BLOCK),
                          buf_w.reshape(n_blocks, EXPERT_BLOCK), block_e))
    return jax.ops.segment_sum(out.reshape(cap, d), buf_tok, num_segments=t)


def setup_inputs(seed: int = 0) -> dict:
    key = jax.random.key(seed)
    ks = jax.random.split(key, 24)
    f32 = jnp.float32
    n = lambda k, shp, s: jax.random.normal(k, shp, f32) * s
    L, D = DEPTH, D_MODEL
    return {
        "x": n(ks[0], (BATCH, SEQ, D), 1.0),
        "c": n(ks[1], (BATCH, D), 1.0),
        "w_ada": n(ks[2], (L, D, N_MOD * D), 0.5 * D ** -0.5),
        "b_ada": n(ks[3], (L, N_MOD * D), 0.01),
        "g_norm_mix": 1.0 + n(ks[4], (L, D), 0.02),
        "w_in": n(ks[5], (L, D, IN_COLS), D ** -0.5),
        "q_norm_g": 1.0 + n(ks[6], (L, HEAD_DIM), 0.02),
        "k_norm_g": 1.0 + n(ks[7], (L, HEAD_DIM), 0.02),
        "conv_w": n(ks[8], (L, CONV_K, CONV_WIDTH), CONV_K ** -0.5),
        "w_attn_o": n(ks[9], (L, ATTN_WIDTH, D), ATTN_WIDTH ** -0.5),
        "w_conv_o": n(ks[10], (L, CONV_WIDTH, D), CONV_WIDTH ** -0.5),
        "w_out": n(ks[11], (L, D, D), D ** -0.5),
        "g_norm_ffn": 1.0 + n(ks[12], (L, D), 0.02),
        "w_router": n(ks[13], (L, D, N_EXPERTS), D ** -0.5),
        "router_bias": n(ks[14], (L, N_EXPERTS), 0.01),
        "w_gate_e": n(ks[15], (L, N_EXPERTS, D, EXPERT_FF), D ** -0.5),
        "w_up_e": n(ks[16], (L, N_EXPERTS, D, EXPERT_FF), D ** -0.5),
        "w_down_e": n(ks[17], (L, N_EXPERTS, EXPERT_FF, D), EXPERT_FF ** -0.5),
        "w_gate_s": n(ks[18], (L, D, SHARED_FF), D ** -0.5),
        "w_up_s": n(ks[19], (L, D, SHARED_FF), D ** -0.5),
        "w_down_s": n(ks[20], (L, SHARED_FF, D), SHARED_FF ** -0.5),
    }


def reference(x, c, w_ada, b_ada, g_norm_mix, w_in, q_norm_g, k_norm_g, conv_w, w_attn_o,
              w_conv_o, w_out, g_norm_ffn, w_router, router_bias, w_gate_e, w_up_e, w_down_e,
              w_gate_s, w_up_s, w_down_s):
    b, s, d = x.shape
    for l in range(DEPTH):
        mod = jax.nn.silu(c) @ w_ada[l] + b_ada[l]
        sh_a, sc_a, gt_a, sh_m, sc_m, gt_m = jnp.split(mod[:, None, :], N_MOD, axis=-1)
        h = rms_norm(x, g_norm_mix[l]) * (1 + sc_a) + sh_a
        x = x + gt_a * token_mixer(h, w_in[l], q_norm_g[l], k_norm_g[l], conv_w[l],
                                   w_attn_o[l], w_conv_o[l], w_out[l])
        h = rms_norm(x, g_norm_ffn[l]) * (1 + sc_m) + sh_m
        hf = h.reshape(b * s, d)
        idx, w = route(hf, w_router[l], router_bias[l])
        routed = routed_experts(hf, idx, w, w_gate_e[l], w_up_e[l], w_down_e[l])
        shared = (jax.nn.silu(hf @ w_gate_s[l]) * (hf @ w_up_s[l])) @ w_down_s[l]
        x = x + gt_m * (routed + shared).reshape(b, s, d)
    return x
```

```python
import numpy as np
import concourse.bass as bass
import concourse.mybir as mybir
from concourse.bass_utils import run_bass_kernel_spmd

F32 = mybir.dt.float32
BF16 = mybir.dt.bfloat16
AF = mybir.ActivationFunctionType
ALU = mybir.AluOpType
AX = mybir.AxisListType

NCORES = 8
D = 1024
S = 2048
NB = 2
NE = 256
EPS = 1e-6
NDMA_SEM = 8


class Buf:
    __slots__ = ("w", "r", "rd")

    def __init__(self):
        self.w = None
        self.r = {}
        self.rd = []


class Op:
    __slots__ = ("eng", "fn", "deps", "dma", "signal", "sem", "val")


class Prog:
    ENGS = ("sp", "act", "dve", "pool", "pe")

    def __init__(self):
        self.ops = {e: [] for e in self.ENGS}
        self.ndma = {e: [] for e in self.ENGS}
        self.last = {e: None for e in self.ENGS}
        self.pend = {e: [] for e in self.ENGS}
        self.dma_since = []

    def _add(self, eng, fn, reads, writes, dma):
        op = Op()
        op.eng, op.fn, op.dma, op.signal, op.sem, op.val = eng, fn, dma, dma, None, None
        deps = set()
        war = set()
        for b in reads:
            if b.w is not None:
                deps.add(b.w)
        for b in writes:
            if b.w is not None:
                deps.add(b.w)
            war.update(b.r.values())
            war.update(b.rd)
        war.update(self.pend[eng])
        self.pend[eng] = []
        if dma:
            q = self.ndma[eng]
            if len(q) >= NDMA_SEM:
                war.add(q[len(q) - NDMA_SEM])
            q.append(op)
            self.dma_since.append(op)
        deps.discard(None)
        war.discard(None)
        keep = [d for d in deps if d.dma or d.eng != eng or (eng != "pe" and not dma)]
        keep += [d for d in war if (d.dma or d.eng != eng) and d not in deps]
        op.deps = keep
        for d in op.deps:
            d.signal = True
        for b in reads:
            if dma:
                b.rd.append(op)
            else:
                b.r[eng] = op
        for b in writes:
            b.w = op
            b.r = {}
            b.rd = []
        self.ops[eng].append(op)
        if not dma:
            self.last[eng] = op
        return op

    def op(self, eng, fn, reads=(), writes=()):
        return self._add(eng, fn, reads, writes, False)

    def dma(self, eng, out, in_, reads=(), writes=()):
        return self._add(eng, lambda e: e.dma_start(out=out, in_=in_), reads, writes, True)

    def barrier(self):
        front = [self.last[e] for e in self.ENGS if self.last[e] is not None] + self.dma_since
        self.dma_since = []
        for e in self.ENGS:
            self.pend[e] = list(front)

    def emit(self, nc):
        sems = {}
        import contextlib
        stack = contextlib.ExitStack()
        for e in self.ENGS:
            sems[("c", e)] = stack.enter_context(nc.semaphore("c_" + e))
            for i in range(NDMA_SEM):
                sems[("d", e, i)] = stack.enter_context(nc.semaphore("d_%s_%d" % (e, i)))
        for e in self.ENGS:
            cnt = 0
            for op in self.ops[e]:
                if op.dma:
                    continue
                if op.signal:
                    cnt += 1
                    op.sem, op.val = ("c", e), cnt
            for k, op in enumerate(self.ndma[e]):
                op.sem, op.val = ("d", e, k % NDMA_SEM), 16 * (k // NDMA_SEM + 1)

        def run(e, h):
            waited = {}
            for op in self.ops[e]:
                need = {}
                for d in op.deps:
                    if need.get(d.sem, 0) < d.val:
                        need[d.sem] = d.val
                for sm, v in need.items():
                    if waited.get(sm, 0) < v:
                        h.wait_ge(sems[sm], v)
                        waited[sm] = v
                ins = op.fn(h) if op.fn is not None else None
                if op.signal and ins is not None:
                    ins.then_inc(sems[op.sem], 16 if op.dma else 1)

        with nc.Block() as block:
            block.sync(lambda h: run("sp", h))
            block.scalar(lambda h: run("act", h))
            block.vector(lambda h: run("dve", h))
            block.gpsimd(lambda h: run("pool", h))
            block.tensor(lambda h: run("pe", h))
        stack.close()


def build_program(mode="full", n_exp=NE + 1, dbg=False):
    nc = bass.Bass("TRN2", target_bir_lowering=False)
    p = Prog()

    def din(name, shape):
        return nc.dram_tensor(name, shape, F32, kind="ExternalInput").ap()

    d_xs = din("xs", [NB * S, D])
    d_cT = din("cT", [128, 8, NB])
    d_wada = din("wada", [6 * 128, 8, 1024])
    d_bada = din("bada", [128, 48])
    d_gmix = din("gmix", [128, 8])
    d_gffn = din("gffn", [128, 8])
    d_win = din("win", [128, 8, 4352])
    d_g10 = din("g10", [128, 640])
    d_convw = din("convw", [128, 4, 3])
    d_wao = din("wao", [64, 8, 1024])
    d_wco = din("wco", [128, 4, 1024])
    d_wout = din("wout", [128, 8, 1024])
    d_wr = din("wr", [128, 8, 256])
    d_rbias = din("rbias", [128, 256])
    exps = (list(range(n_exp - 1)) + [NE]) if n_exp <= NE else list(range(NE + 1))
    if mode == "mixer":
        exps = [NE]
    d_wgu = din("wgu", [len(exps) * 128, 8, 512])
    d_wd = din("wd", [len(exps) * 128, 2, 1024])
    d_ccss = din("ccss", [128, 16, 128])
    d_ident = din("ident", [128, 128])
    d_out = nc.dram_tensor("out", [NB * S, D], F32, kind="ExternalOutput").ap()
    d_xscr = nc.dram_tensor("xscr", [NB * S, D], F32, kind="Internal").ap()

    def dump(name, ap):
        if not dbg:
            return
        shp = list(ap.shape)
        p.barrier()
        dd = nc.dram_tensor("dbg_" + name, shp, F32, kind="ExternalOutput").ap()
        p.dma("pool", dd, ap, writes=[Buf()])
        p.barrier()

    cnt = [0]

    def sb(shape, dtype, off):
        cnt[0] += 1
        return nc.alloc_sbuf_tensor_at("t%d" % cnt[0], list(shape), dtype, offset=off + 16640, align_bytes=4).ap()

    ident_b = sb([128, 128], BF16, 0)
    ident_f = sb([128, 128], F32, 256)
    g10 = sb([128, 640], F32, 768)
    rbias = sb([128, 256], F32, 3328)
    bada = sb([128, 48], F32, 4352)
    gmix = sb([128, 8], F32, 4544)
    gffn = sb([128, 8], F32, 4576)
    convw = sb([128, 4, 3], F32, 4608)
    cT = sb([128, 8, NB], F32, 4672)
    scT = sb([128, 8, NB], BF16, 4736)
    modT = sb([128, 48, NB], F32, 4768)
    Aa = sb([128, 8, NB], F32, 5152)
    Am = sb([128, 8, NB], F32, 5216)
    epst = sb([128, 1], F32, 5280)
    onesf = sb([128, 64], F32, 5312)
    Gta = sb([128, 1024], F32, 5632)
    Gtm = sb([128, 1024], F32, 9728)
    sm_ss = [sb([128, 1], F32, 13824 + 4 * i) for i in range(2)]
    sm_ms = [sb([128, 1], F32, 13856 + 4 * i) for i in range(2)]
    sm_rs = [sb([128, 1], F32, 13888 + 4 * i) for i in range(2)]
    sm_ssq = [sb([128, 10], F32, 13920 + 40 * i) for i in range(2)]
    sm_msq = [sb([128, 10], F32, 14016 + 40 * i) for i in range(2)]
    sm_rq = [sb([128, 10], F32, 14112 + 40 * i) for i in range(2)]
    bcl = [sb([128, 128], F32, 14336 + 512 * i) for i in range(2)]
    r_m8 = [sb([128, 8, 8], F32, 15360 + 256 * i) for i in range(2)]
    r_gs = [sb([128, 8], F32, 15872 + 32 * i) for i in range(2)]
    r_gm8 = [sb([128, 8], F32, 15936 + 32 * i) for i in range(2)]
    r_gmask = [sb([128, 8], F32, 16000 + 32 * i) for i in range(2)]
    r_pen = [sb([128, 8], F32, 16064 + 32 * i) for i in range(2)]
    r_t8 = [sb([128, 8], F32, 16128 + 32 * i) for i in range(2)]
    r_ws = [sb([128, 1], F32, 16192 + 4 * i) for i in range(2)]
    r_rw = [sb([128, 1], F32, 16224 + 4 * i) for i in range(2)]

    OB, OQA, OR1, OYT, OW, OT = 20480, 53248, 86016, 118784, 135168, 159744
    hT = sb([128, 8, S], BF16, OB)
    QT = sb([128, 8, S], BF16, OQA)
    KT = sb([128, 2, S], BF16, OR1)
    Vt = sb([128, 16, 2, 128], BF16, OR1 + 8192)
    mT = sb([128, 8, S], BF16, OR1)
    yT = sb([128, 4, S], BF16, OYT)
    Wout = sb([128, 8, 1024], BF16, OYT)
    acc = sb([128, 16, 1024], F32, OQA)
    Gw = sb([128, 16, 256], F32, OYT)
    wgu = [sb([128, 8, 512], BF16, OW + 12288 * i) for i in range(2)]
    wdn = [sb([128, 2, 1024], BF16, OW + 12288 * i + 8192) for i in range(2)]
    wada_s = [sb([128, 8, 1024], BF16, o) for o in (OW, OT, OT + 16384)]
    xt = [sb([128, 1024], F32, OT + 4096 * i) for i in range(2)]
    xnw = [sb([128, 1024], F32, OT + 8192 + 4096 * i) for i in range(2)]
    xn = [sb([128, 1024], BF16, OT + 16384 + 2048 * i) for i in range(2)]
    Wqkv = sb([128, 8, 768], BF16, OW)
    ccss = sb([128, 16, 128], F32, OT + 32768)
    s2 = []
    for i in range(2):
        o = OT + 8192 + 11520 * i
        s2.append(dict(sq=sb([128, 640], F32, o), qn=sb([128, 640], F32, o + 2560), t1=sb([128, 640], F32, o + 5120),
                       t2=sb([128, 640], F32, o + 7680), qr=sb([128, 640], BF16, o + 10240)))
    Wbcx = sb([128, 8, 1536], BF16, OW)
    ub = sb([128, 2050], F32, OT + 12288)
    Bsb = sb([128, 2048], F32, OT + 20512)
    cacc = sb([128, 2048], F32, OT + 28704)
    ctmp = [sb([128, 512], F32, 16384 + 2048 * i) for i in range(2)]
    PT = [sb([128, 512], BF16, OT + 1024 * i) for i in range(4)]
    Ou = [sb([128, 512], F32, OT + 4096 + 2048 * i) for i in range(2)]
    rsb = [sb([128, 512], F32, OT + 8192 + 2048 * i) for i in range(2)]
    Wga = [sb([128, 8, 128], BF16, OW + 7168 * i) for i in range(2)]
    Wgc = [sb([128, 8, 128], BF16, OW + 7168 * i + 2048) for i in range(2)]
    Waoc = [sb([128, 8, 128], BF16, OW + 7168 * i + 4096) for i in range(2)]
    Wcoc = [sb([128, 4, 128], BF16, OW + 7168 * i + 6144) for i in range(2)]
    s5 = []
    for i in range(2):
        o = OT + 8192 * i
        s5.append(dict(sa=sb([128, 512], F32, o), sc=sb([128, 512], F32, o + 2048), m1=sb([128, 512], F32, o + 4096),
                       m2=sb([128, 512], F32, o + 6144)))
    Wr = sb([128, 8, 256], BF16, OT)
    s7 = []
    for i in range(2):
        o = OT + 4096 + 5120 * i
        s7.append(dict(s=sb([128, 256], F32, o), bsd=sb([128, 256], F32, o + 1024), msk=sb([128, 256], F32, o + 2048),
                       sel=sb([128, 256], F32, o + 3072), w=sb([128, 256], F32, o + 4096)))
    sg = [sb([128, 2, 512], F32, OT + 16384 + 4096 * i) for i in range(2)]
    hid = [sb([128, 2, 512], BF16, OT + 24576 + 2048 * i) for i in range(2)]
    xt3 = [sb([128, 1024], F32, OT + 4096 * i) for i in range(4)]
    ot = [sb([128, 1024], F32, OW + 4096 * i) for i in range(4)]

    banks = [nc.alloc_psum_tensor("pb%d" % i, [128, 512], F32).ap() for i in range(8)]
    banks_bf = [b.bitcast(BF16) for b in banks]
    bbuf = [Buf() for _ in range(8)]
    rot = {}

    def pbank(group, idxs):
        k = rot.get(group, 0)
        rot[group] = k + 1
        i = idxs[k % len(idxs)]
        return banks[i], banks_bf[i], bbuf[i]

    def B():
        return Buf()

    b_const = B()
    for dst, src in ((ident_f, d_ident), (g10, d_g10), (rbias, d_rbias), (bada, d_bada), (gmix, d_gmix), (gffn, d_gffn),
                     (convw, d_convw), (cT, d_cT)):
        p.dma("sp", dst, src, writes=[B()])
    p.barrier()
    p.op("dve", lambda e: e.tensor_copy(out=ident_b, in_=ident_f), writes=[b_const])
    p.op("dve", lambda e: e.memset(epst, EPS), writes=[b_const])
    p.op("dve", lambda e: e.memset(onesf, 1.0), writes=[b_const])
    p.op("dve", lambda e: e.tensor_scalar_mul(out=g10[:, 0:512], in0=g10[:, 0:512], scalar1=0.125), writes=[b_const])
    p.op("act", lambda e: e.activation(out=scT, in_=cT, func=AF.Silu), writes=[b_const])
    p.barrier()
    pm, _, pmb = pbank("s0", [0])
    wb = [B() for _ in range(3)]
    for m in range(6):
        sl = m % 3
        p.dma("pool", wada_s[sl], d_wada[m * 128:(m + 1) * 128], writes=[wb[sl]])

        def f(e, m=m, sl=sl):
            ins = None
            for j in range(8):
                for kc in range(8):
                    c0 = ((m * 8 + j) * NB)
                    ins = e.matmul(pm[:, c0:c0 + NB], lhsT=wada_s[sl][:, kc, j * 128:(j + 1) * 128], rhs=scT[:, kc, :],
                                   start=(kc == 0), stop=(kc == 7))
            return ins
        p.op("pe", f, reads=[wb[sl]], writes=[pmb])
    b_mod = B()
    p.op("dve", lambda e: e.tensor_tensor(out=modT, in0=pm[:, 0:48 * NB].rearrange("p (j b) -> p j b", b=NB),
                                          in1=bada.unsqueeze(2).to_broadcast([128, 48, NB]), op=ALU.add),
         reads=[pmb], writes=[b_mod])
    for (A_, g_, base) in ((Aa, gmix, 8), (Am, gffn, 32)):
        p.op("dve", lambda e, A_=A_, base=base: e.tensor_scalar_add(out=A_, in0=modT[:, base:base + 8, :], scalar1=1.0),
             reads=[b_mod], writes=[b_mod])
        p.op("dve", lambda e, A_=A_, g_=g_: e.tensor_tensor(out=A_, in0=A_, in1=g_.unsqueeze(2).to_broadcast([128, 8, NB]),
                                                           op=ALU.mult), reads=[b_mod], writes=[b_mod])
    p.barrier()
    dump("mod", modT)

    sl_b = dict(ss=[B(), B()], xn=[B(), B()])

    def norm_transpose(src, src_buf, sl, A_, base_s, b, tt, dst, dst_bufs):
        ssb, xnb = sl_b["ss"][sl], sl_b["xn"][sl]
        p.op("act", lambda e: e.memzero(sm_ss[sl]), writes=[ssb])
        p.op("act", lambda e: e.activation(out=xn[sl], in_=src, func=AF.Square, accum_out=sm_ss[sl]),
             reads=[src_buf], writes=[ssb, xnb])
        p.op("dve", lambda e: e.tensor_scalar(out=sm_ms[sl], in0=sm_ss[sl], scalar1=1.0 / D, scalar2=EPS,
                                              op0=ALU.mult, op1=ALU.add), reads=[ssb], writes=[ssb])
        p.op("act", lambda e: e.activation(out=sm_ms[sl], in_=sm_ms[sl], func=AF.Sqrt), reads=[ssb], writes=[ssb])
        p.op("dve", lambda e: e.reciprocal(out=sm_rs[sl], in_=sm_ms[sl]), reads=[ssb], writes=[ssb])
        p.op("act", lambda e: e.activation(out=xn[sl], in_=src, func=AF.Copy, scale=sm_rs[sl]),
             reads=[ssb, src_buf], writes=[xnb])
        _, pb, pbb = pbank("tr", [0, 1])

        def ft(e):
            ins = None
            for kc in range(8):
                ins = e.transpose(out=pb[:, kc * 128:(kc + 1) * 128], in_=xn[sl][:, kc * 128:(kc + 1) * 128],
                                  identity=ident_b)
            return ins
        p.op("pe", ft, reads=[xnb], writes=[pbb])

        def fm(e):
            ins = None
            for kc in range(8):
                ins = e.tensor_scalar(out=dst[:, kc, tt * 128:(tt + 1) * 128], in0=pb[:, kc * 128:(kc + 1) * 128],
                                      scalar1=A_[:, kc, b:b + 1], scalar2=modT[:, base_s + kc, b:b + 1],
                                      op0=ALU.mult, op1=ALU.add)
            return ins
        p.op("dve", fm, reads=[pbb], writes=dst_bufs)

    b_xscr = [[B() for _ in range(16)] for _ in range(NB)]
    out_bufs = []

    for b in range(NB):
        row0 = b * S
        b_gt = B()
        bclb = [B(), B()]
        k = 0
        for (dst, base) in ((Gta, 16), (Gtm, 40)):
            for half in range(2):
                pbk, _, pbb = pbank("gt", [2, 3])
                for kk in range(4):
                    kc = half * 4 + kk
                    sl = k % 2
                    k += 1
                    bb = bclb[sl]
                    p.op("dve", lambda e, sl=sl, base=base, kc=kc, b=b: e.tensor_copy(
                        out=bcl[sl], in_=modT[:, base + kc, b:b + 1].to_broadcast([128, 128])), writes=[bb])
                    p.op("pe", lambda e, sl=sl, kk=kk, pbk=pbk: e.matmul(pbk[:, kk * 128:(kk + 1) * 128], lhsT=bcl[sl],
                                                                       rhs=ident_f, start=True, stop=True),
                         reads=[bb], writes=[pbb])
                p.op("act", lambda e, dst=dst, half=half, pbk=pbk: e.activation(
                    out=dst[:, half * 512:(half + 1) * 512], in_=pbk, func=AF.Copy), reads=[pbb], writes=[b_gt])
        p.barrier()

        b_w2 = B()
        p.dma("pool", Wqkv, d_win[:, :, 0:768], writes=[b_w2])
        b_tab = B()
        p.dma("sp", ccss, d_ccss, writes=[b_tab])

        b_hT = [B() for _ in range(16)]
        xb = [B(), B()]
        for tt in range(16):
            sl = tt % 2
            p.dma("sp", xt[sl], d_xs[row0 + tt * 128: row0 + (tt + 1) * 128, :], writes=[xb[sl]])
            norm_transpose(xt[sl], xb[sl], sl, Aa, 0, b, tt, hT, [b_hT[tt]])
        p.barrier()
        if b == 0:
            dump("hT", hT)
            dump("Gta", Gta)

        b_w = b_w2
        p.op("dve", lambda e: e.memset(Vt[:, :, :, 64:128], 1.0), writes=[b_tab])
        p.op("dve", lambda e: e.memset(QT[64:128], 0.0), writes=[b_tab])
        p.op("dve", lambda e: e.memset(KT[64:128], 0.0), writes=[b_tab])
        b_QT = [[B() for _ in range(4)] for _ in range(8)]
        b_KT = [B() for _ in range(16)]
        b_V = [B() for _ in range(16)]
        s2b = [B(), B()]
        def emit_qkv(tt):
            tok = slice(tt * 128, (tt + 1) * 128)
            bq, _, bqb = pbank("s2", [2, 3, 4, 5])
            bkv, _, bkvb = pbank("s2", [2, 3, 4, 5])

            def fq(e, bq=bq, bkv=bkv, tok=tok):
                ins = None
                for kc in range(8):
                    ins = e.matmul(bq, lhsT=hT[:, kc, tok], rhs=Wqkv[:, kc, 0:512], start=(kc == 0), stop=(kc == 7))
                for kc in range(8):
                    ins = e.matmul(bkv[:, 0:256], lhsT=hT[:, kc, tok], rhs=Wqkv[:, kc, 512:768], start=(kc == 0),
                                   stop=(kc == 7))
                return ins
            p.op("pe", fq, reads=[b_hT[tt], b_w], writes=[bqb, bkvb])
            return bq, bqb, bkv, bkvb
        nxt_qkv = emit_qkv(0)
        for tt in range(16):
            sl = tt % 2
            t_ = s2[sl]
            tb = s2b[sl]
            tok = slice(tt * 128, (tt + 1) * 128)
            bq, bqb, bkv, bkvb = nxt_qkv
            if tt + 1 < 16:
                nxt_qkv = emit_qkv(tt + 1)
            p.op("act", lambda e, t_=t_, bq=bq: e.activation(out=t_["sq"][:, 0:512], in_=bq, func=AF.Square),
                 reads=[bqb], writes=[tb])
            p.op("act", lambda e, t_=t_, bkv=bkv: e.activation(out=t_["sq"][:, 512:640], in_=bkv[:, 0:128], func=AF.Square),
                 reads=[bkvb], writes=[tb])
            p.op("act", lambda e, bkv=bkv, tt=tt: e.activation(
                out=Vt[:, tt, :, 0:64], in_=bkv[:, 128:256].rearrange("p (g d) -> p g d", d=64), func=AF.Copy),
                reads=[bkvb, b_tab], writes=[b_V[tt]])
            p.op("dve", lambda e, t_=t_, sl=sl: e.tensor_reduce(out=sm_ssq[sl], in_=t_["sq"].rearrange("p (h d) -> p h d", d=64),
                                                                axis=AX.X, op=ALU.add), reads=[tb], writes=[tb])
            p.op("dve", lambda e, sl=sl: e.tensor_scalar(out=sm_msq[sl], in0=sm_ssq[sl], scalar1=1.0 / 64, scalar2=EPS,
                                                         op0=ALU.mult, op1=ALU.add), reads=[tb], writes=[tb])
            p.op("act", lambda e, sl=sl: e.activation(out=sm_msq[sl], in_=sm_msq[sl], func=AF.Sqrt), reads=[tb], writes=[tb])
            p.op("dve", lambda e, sl=sl: e.reciprocal(out=sm_rq[sl], in_=sm_msq[sl]), reads=[tb], writes=[tb])
            p.op("dve", lambda e, t_=t_, sl=sl, bq=bq: e.tensor_tensor(
                out=t_["qn"][:, 0:512].rearrange("p (h d) -> p h d", d=64), in0=bq.rearrange("p (h d) -> p h d", d=64),
                in1=sm_rq[sl][:, 0:8].unsqueeze(2).to_broadcast([128, 8, 64]), op=ALU.mult), reads=[tb, bqb], writes=[tb])
            p.op("dve", lambda e, t_=t_, sl=sl, bkv=bkv: e.tensor_tensor(
                out=t_["qn"][:, 512:640].rearrange("p (h d) -> p h d", d=64),
                in0=bkv[:, 0:128].rearrange("p (h d) -> p h d", d=64),
                in1=sm_rq[sl][:, 8:10].unsqueeze(2).to_broadcast([128, 2, 64]), op=ALU.mult), reads=[tb, bkvb], writes=[tb])
            p.op("dve", lambda e, t_=t_: e.tensor_tensor(out=t_["qn"], in0=t_["qn"], in1=g10, op=ALU.mult),
                 reads=[tb], writes=[tb])
            p.op("dve", lambda e, t_=t_, tt=tt: e.tensor_tensor(
                out=t_["t1"].rearrange("p (h d) -> p h d", d=64), in0=t_["qn"].rearrange("p (h d) -> p h d", d=64),
                in1=ccss[:, tt, 0:64].unsqueeze(1).to_broadcast([128, 10, 64]), op=ALU.mult), reads=[tb, b_tab], writes=[tb])
            for s_ in range(2):
                def fr(e, t_=t_, tt=tt, s_=s_):
                    qv = t_["qn"].rearrange("p (h f s c) -> p h f s c", h=10, f=2, s=2, c=16)
                    tv = t_["t2"].rearrange("p (h f s c) -> p h f s c", h=10, f=2, s=2, c=16)
                    ssv = ccss[:, tt, 64:128].rearrange("p (f s c) -> p f s c", f=2, s=2, c=16)
                    return e.tensor_tensor(out=tv[:, :, :, s_, :], in0=qv[:, :, :, 1 - s_, :],
                                           in1=ssv[:, :, s_, :].unsqueeze(1).to_broadcast([128, 10, 2, 16]), op=ALU.mult)
                p.op("dve", fr, reads=[tb, b_tab], writes=[tb])
            p.op("dve", lambda e, t_=t_: e.tensor_tensor(out=t_["qr"], in0=t_["t1"], in1=t_["t2"], op=ALU.add),
                 reads=[tb], writes=[tb])
            _, ptq, ptqb = pbank("tr", [0, 1])
            _, ptk, ptkb = pbank("s2k", [6, 7])

            def ftq(e, t_=t_, ptq=ptq, ptk=ptk):
                ins = None
                for h in range(8):
                    ins = e.transpose(out=ptq[0:64, h * 128:(h + 1) * 128], in_=t_["qr"][:, h * 64:(h + 1) * 64],
                                      identity=ident_b)
                for g in range(2):
                    ins = e.transpose(out=ptk[0:64, g * 128:(g + 1) * 128],
                                      in_=t_["qr"][:, 512 + g * 64:512 + (g + 1) * 64], identity=ident_b)
                return ins
            p.op("pe", ftq, reads=[tb], writes=[ptqb, ptkb])
            p.op("act", lambda e, ptq=ptq, tok=tok: e.activation(
                out=QT[0:64, :, tok], in_=ptq[0:64, 0:1024].rearrange("p (h t) -> p h t", t=128), func=AF.Copy),
                reads=[ptqb], writes=[b_QT[h][tt // 4] for h in range(8)])
            p.op("dve", lambda e, ptk=ptk, tok=tok: e.tensor_copy(
                out=KT[0:64, :, tok], in_=ptk[0:64, 0:256].rearrange("p (h t) -> p h t", t=128)),
                reads=[ptkb], writes=[b_KT[tt]])
        p.barrier()
        if b == 0:
            dump("QT", QT[0:64])
            dump("KT", KT[0:64])
            dump("Vt", Vt)

        b_w = B()
        p.dma("pool", Wbcx, d_win[:, :, 768:2304], writes=[b_w])
        b_u = B()
        b_Bs = B()
        b_yT = [B() for _ in range(4)]
        cb = [B(), B()]
        p.op("dve", lambda e: e.memset(ub[:, 0:1], 0.0), writes=[b_u])
        p.op("dve", lambda e: e.memset(ub[:, 2049:2050], 0.0), writes=[b_u])
        conv_q = []
        k = 0
        for cc in range(4):
            for tq in range(4):
                sl = k % 2
                k += 1
                for j in (1, 2, 0):

                    def cg(cc=cc, tq=tq, j=j, sl=sl):
                        tq5 = slice(tq * 512, (tq + 1) * 512)
                        bk, _, bkb = pbank("s3", [6])
                        off = j * 512 + cc * 128

                        def fc(e):
                            ins = None
                            for kc in range(8):
                                ins = e.matmul(bk, lhsT=Wbcx[:, kc, off:off + 128], rhs=hT[:, kc, tq5],
                                               start=(kc == 0), stop=(kc == 7))
                            return ins
                        p.op("pe", fc, reads=[b_w] + b_hT[tq * 4:(tq + 1) * 4], writes=[bkb])
                        if j == 1:
                            p.op("dve", lambda e: e.tensor_copy(out=ctmp[sl], in_=bk), reads=[bkb], writes=[cb[sl]])
                        elif j == 2:
                            p.op("dve", lambda e: e.tensor_tensor(out=ub[:, 1 + tq * 512:1 + (tq + 1) * 512], in0=ctmp[sl],
                                                                  in1=bk, op=ALU.mult), reads=[cb[sl], bkb], writes=[b_u])
                        else:
                            p.op("dve", lambda e: e.tensor_copy(out=Bsb[:, tq5], in_=bk), reads=[bkb], writes=[b_Bs])
                    conv_q.append(cg)

            def ctaps(cc=cc):
                p.op("dve", lambda e: e.tensor_scalar_mul(out=cacc, in0=ub[:, 0:2048], scalar1=convw[:, cc, 0:1]),
                     reads=[b_u], writes=[b_u])
                p.op("dve", lambda e: e.scalar_tensor_tensor(out=cacc, in0=ub[:, 1:2049], scalar=convw[:, cc, 1:2],
                                                             in1=cacc, op0=ALU.mult, op1=ALU.add), reads=[b_u], writes=[b_u])
                p.op("dve", lambda e: e.scalar_tensor_tensor(out=cacc, in0=ub[:, 2:2050], scalar=convw[:, cc, 2:3],
                                                             in1=cacc, op0=ALU.mult, op1=ALU.add), reads=[b_u], writes=[b_u])
                p.op("dve", lambda e: e.tensor_tensor(out=yT[:, cc, :], in0=cacc, in1=Bsb, op=ALU.mult),
                     reads=[b_u, b_Bs], writes=[b_yT[cc], b_u, b_Bs])
            conv_q.append(ctaps)
        conv_slot = 0

        ptb = [B() for _ in range(4)]
        oub = [B(), B()]
        rsbb = [B(), B()]
        it = 0
        kpt = 0
        pending_ep = None
        for g in range(2):
            for r in range(4):
                h = g * 4 + r
                for qc in range(4):
                    sl = it % 2
                    it += 1
                    q5 = slice(qc * 512, (qc + 1) * 512)
                    bO, _, bOb = pbank("s4o", [0, 1])

                    def emit_S(kt, g=g, h=h, q5=q5, qc=qc):
                        bS, _, bSb = pbank("s4s", [2, 3, 4, 5])
                        p.op("pe", lambda e, bS=bS, kt=kt, g=g, h=h, q5=q5: e.matmul(
                            bS, lhsT=KT[:, g, kt * 128:(kt + 1) * 128], rhs=QT[:, h, q5], start=True, stop=True),
                             reads=[b_KT[kt], b_QT[h][qc], b_tab], writes=[bSb])
                        return bS, bSb
                    cur = [emit_S(0), emit_S(1)]
                    for grp in range(8):
                        nxt = [emit_S(2 * grp + 2), emit_S(2 * grp + 3)] if grp < 7 else []
                        if grp == 4 and pending_ep is not None:
                            pending_ep()
                            pending_ep = None
                        slots = []
                        for i in range(2):
                            ps_ = kpt % 4
                            kpt += 1
                            slots.append(ps_)
                            p.op("act", lambda e, c_=cur[i], ps_=ps_: e.activation(out=PT[ps_], in_=c_[0], func=AF.Exp),
                                 reads=[cur[i][1]], writes=[ptb[ps_]])
                        for i in range(2):
                            kt = 2 * grp + i
                            ps_ = slots[i]
                            p.op("pe", lambda e, kt=kt, ps_=ps_, bO=bO, g=g: e.matmul(bO, lhsT=Vt[:, kt, g, :], rhs=PT[ps_],
                                                                                    start=(kt == 0), stop=(kt == 15)),
                                 reads=[ptb[ps_], b_V[kt]], writes=[bOb])
                        cur = nxt
                        conv_slot += 1
                        if conv_q and conv_slot % 4 == 2:
                            conv_q.pop(0)()
                    p.op("dve", lambda e, sl=sl, bO=bO: e.reciprocal(out=rsb[sl][64:65, :], in_=bO[64:65, :]),
                         reads=[bOb], writes=[rsbb[sl]])
                    p.op("dve", lambda e, sl=sl, bO=bO: e.tensor_copy(out=Ou[sl][0:64, :], in_=bO[0:64, :]),
                         reads=[bOb], writes=[oub[sl]])

                    def epilogue(sl=sl, h=h, q5=q5, qc=qc):
                        bBc, _, bBcb = pbank("s4b", [7])
                        p.op("pe", lambda e: e.matmul(bBc[0:64, :], lhsT=onesf[64:65, 0:64], rhs=rsb[sl][64:65, :],
                                                      start=True, stop=True), reads=[rsbb[sl]], writes=[bBcb])
                        p.op("dve", lambda e: e.tensor_tensor(out=QT[0:64, h, q5], in0=Ou[sl][0:64, :], in1=bBc[0:64, :],
                                                              op=ALU.mult), reads=[oub[sl], bBcb], writes=[b_QT[h][qc]])
                    pending_ep = epilogue
        if pending_ep is not None:
            pending_ep()
            pending_ep = None
        while conv_q:
            conv_q.pop(0)()
        p.barrier()
        if b == 0:
            dump("yT", yT)
            dump("attnT", QT[0:64])

        wsb = [B(), B()]
        b_mT = [B() for _ in range(4)]
        s5b = [B(), B()]
        it = 0
        for dc in range(8):
            ws = dc % 2
            d5 = slice(dc * 128, (dc + 1) * 128)
            p.dma("pool", Wga[ws], d_win[:, :, 2304 + dc * 128:2304 + (dc + 1) * 128], writes=[wsb[ws]])
            p.dma("pool", Wgc[ws], d_win[:, :, 3328 + dc * 128:3328 + (dc + 1) * 128], writes=[wsb[ws]])
            p.dma("pool", Waoc[ws][0:64], d_wao[:, :, d5], writes=[wsb[ws]])
            p.dma("pool", Wcoc[ws], d_wco[:, :, d5], writes=[wsb[ws]])
            for tq in range(4):
                sl = it % 2
                it += 1
                t_ = s5[sl]
                tq5 = slice(tq * 512, (tq + 1) * 512)
                bks = [pbank("s5", [0, 1, 2, 3, 4, 5, 6, 7]) for _ in range(4)]

                def f5(e, bks=bks, ws=ws, tq5=tq5):
                    ins = None
                    for h in range(8):
                        ins = e.matmul(bks[0][0], lhsT=Waoc[ws][0:64, h, :], rhs=QT[0:64, h, tq5], start=(h == 0), stop=(h == 7))
                    for cc in range(4):
                        ins = e.matmul(bks[1][0], lhsT=Wcoc[ws][:, cc, :], rhs=yT[:, cc, tq5], start=(cc == 0), stop=(cc == 3))
                    for kc in range(8):
                        ins = e.matmul(bks[2][0], lhsT=Wga[ws][:, kc, :], rhs=hT[:, kc, tq5], start=(kc == 0), stop=(kc == 7))
                    for kc in range(8):
                        ins = e.matmul(bks[3][0], lhsT=Wgc[ws][:, kc, :], rhs=hT[:, kc, tq5], start=(kc == 0), stop=(kc == 7))
                    return ins
                p.op("pe", f5, reads=[wsb[ws]] + [b_QT[h][tq] for h in range(8)] + b_yT + b_hT[tq * 4:(tq + 1) * 4],
                     writes=[x[2] for x in bks])
                p.op("act", lambda e, t_=t_, bks=bks: e.activation(out=t_["sa"], in_=bks[2][0], func=AF.Sigmoid),
                     reads=[bks[2][2]], writes=[s5b[sl]])
                p.op("act", lambda e, t_=t_, bks=bks: e.activation(out=t_["sc"], in_=bks[3][0], func=AF.Sigmoid),
                     reads=[bks[3][2]], writes=[s5b[sl]])
                p.op("dve", lambda e, t_=t_, bks=bks: e.tensor_tensor(out=t_["m1"], in0=t_["sa"], in1=bks[0][0], op=ALU.mult),
                     reads=[s5b[sl], bks[0][2]], writes=[s5b[sl]])
                p.op("dve", lambda e, t_=t_, bks=bks: e.tensor_tensor(out=t_["m2"], in0=t_["sc"], in1=bks[1][0], op=ALU.mult),
                     reads=[s5b[sl], bks[1][2]], writes=[s5b[sl]])
                p.op("dve", lambda e, t_=t_, dc=dc, tq5=tq5: e.tensor_tensor(out=mT[:, dc, tq5], in0=t_["m1"], in1=t_["m2"],
                                                                           op=ALU.add), reads=[s5b[sl]], writes=[b_mT[tq], s5b[sl]])
        p.barrier()
        if b == 0:
            dump("mT", mT)

        b_w = B()
        p.dma("pool", Wout, d_wout, writes=[b_w])
        b_h2 = [B() for _ in range(16)]
        xb = [B(), B()]
        xnb_ = [B(), B()]
        def emit_s6(tt):
            tok = slice(tt * 128, (tt + 1) * 128)
            bks = [pbank("s6", [2, 3, 4, 5, 6, 7]) for _ in range(2)]

            def f6(e, bks=bks, tok=tok):
                ins = None
                for half in range(2):
                    for kc in range(8):
                        ins = e.matmul(bks[half][0], lhsT=mT[:, kc, tok], rhs=Wout[:, kc, half * 512:(half + 1) * 512],
                                       start=(kc == 0), stop=(kc == 7))
                return ins
            p.op("pe", f6, reads=[b_w, b_mT[tt // 4]], writes=[x[2] for x in bks])
            return bks
        nxt_s6 = emit_s6(0)
        for tt in range(16):
            sl = tt % 2
            tok = slice(tt * 128, (tt + 1) * 128)
            rows = slice(row0 + tt * 128, row0 + (tt + 1) * 128)
            p.dma("sp", xt[sl], d_xs[rows, :], writes=[xb[sl]])
            bks = nxt_s6
            if tt + 1 < 16:
                nxt_s6 = emit_s6(tt + 1)
            for half in range(2):
                hs = slice(half * 512, (half + 1) * 512)
                p.op("dve", lambda e, sl=sl, bks=bks, half=half, hs=hs: e.tensor_tensor(
                    out=xnw[sl][:, hs], in0=bks[half][0], in1=Gta[:, hs], op=ALU.mult), reads=[bks[half][2]], writes=[xnb_[sl]])
            p.op("dve", lambda e, sl=sl: e.tensor_tensor(out=xnw[sl], in0=xnw[sl], in1=xt[sl], op=ALU.add),
                 reads=[xb[sl]], writes=[xnb_[sl]])
            p.dma("sp", d_xscr[rows, :], xnw[sl], reads=[xnb_[sl]], writes=[b_xscr[b][tt]])
            norm_transpose(xnw[sl], xnb_[sl], sl, Am, 24, b, tt, hT, [b_h2[tt]])
        p.barrier()

        if mode == "mixer":
            continue

        b_w = B()
        p.dma("pool", Wr, d_wr, writes=[b_w])
        b_Gw = [B() for _ in range(16)]
        s7b = [B(), B()]
        for tt in range(16):
            sl = tt % 2
            t_ = s7[sl]
            tb = s7b[sl]
            tok = slice(tt * 128, (tt + 1) * 128)
            bR, _, bRb = pbank("s7", [0, 1])

            def f7(e, bR=bR, tok=tok):
                ins = None
                for kc in range(8):
                    ins = e.matmul(bR[:, 0:256], lhsT=hT[:, kc, tok], rhs=Wr[:, kc, :], start=(kc == 0), stop=(kc == 7))
                return ins
            p.op("pe", f7, reads=[b_w, b_h2[tt]], writes=[bRb])
            p.op("act", lambda e, t_=t_, bR=bR: e.activation(out=t_["s"], in_=bR[:, 0:256], func=AF.Sigmoid),
                 reads=[bRb], writes=[tb])
            p.op("dve", lambda e, t_=t_: e.tensor_tensor(out=t_["bsd"], in0=t_["s"], in1=rbias, op=ALU.add), reads=[tb], writes=[tb])

            def D_(fn, extra_w=()):
                p.op("dve", fn, reads=[tb], writes=[tb] + list(extra_w))
            v3 = lambda ap: ap.rearrange("p (g c) -> p g c", c=32)
            for g in range(8):
                D_(lambda e, t_=t_, sl=sl, g=g: e.max(out=r_m8[sl][:, g, :], in_=t_["bsd"][:, g * 32:(g + 1) * 32]))
            D_(lambda e, sl=sl: e.tensor_tensor(out=r_gs[sl], in0=r_m8[sl][:, :, 0], in1=r_m8[sl][:, :, 1], op=ALU.add))
            D_(lambda e, sl=sl: e.max(out=r_gm8[sl], in_=r_gs[sl]))
            D_(lambda e, sl=sl: e.tensor_scalar(out=r_gmask[sl], in0=r_gs[sl], scalar1=r_gm8[sl][:, 3:4], scalar2=None,
                                                op0=ALU.is_ge))
            D_(lambda e, sl=sl: e.tensor_scalar(out=r_pen[sl], in0=r_gmask[sl], scalar1=1.0e4, scalar2=-1.0e4,
                                                op0=ALU.mult, op1=ALU.add))
            D_(lambda e, t_=t_, sl=sl: e.tensor_tensor(out=v3(t_["msk"]), in0=v3(t_["bsd"]),
                                                       in1=r_gmask[sl].unsqueeze(2).to_broadcast([128, 8, 32]), op=ALU.mult))
            D_(lambda e, t_=t_, sl=sl: e.tensor_tensor(out=v3(t_["msk"]), in0=v3(t_["msk"]),
                                                       in1=r_pen[sl].unsqueeze(2).to_broadcast([128, 8, 32]), op=ALU.add))
            D_(lambda e, t_=t_, sl=sl: e.max(out=r_t8[sl], in_=t_["msk"]))
            D_(lambda e, t_=t_, sl=sl: e.tensor_scalar(out=t_["sel"], in0=t_["msk"], scalar1=r_t8[sl][:, 7:8], scalar2=None,
                                                       op0=ALU.is_ge))
            D_(lambda e, t_=t_: e.tensor_tensor(out=t_["w"], in0=t_["s"], in1=t_["sel"], op=ALU.mult))
            D_(lambda e, t_=t_, sl=sl: e.tensor_reduce(out=r_ws[sl], in_=t_["w"], axis=AX.X, op=ALU.add))
            D_(lambda e, sl=sl: e.reciprocal(out=r_rw[sl], in_=r_ws[sl]))
            D_(lambda e, t_=t_, sl=sl, tt=tt: e.tensor_scalar(out=Gw[:, tt, :], in0=t_["w"], scalar1=r_rw[sl], scalar2=2.5,
                                                              op0=ALU.mult, op1=ALU.mult), extra_w=[b_Gw[tt]])

        if b == 0:
            dump("Gw", Gw)

        b_acc = [B() for _ in range(16)]
        web = [B(), B()]
        sgb = [B(), B()]
        hidb = [B(), B()]
        steps = [(i, e, tq) for i, e in enumerate(exps) for tq in range(4)]

        def load(i):
            e = exps[i]
            ws = i % 2
            p.dma("pool", wgu[ws], d_wgu[i * 128:(i + 1) * 128], writes=[web[ws]])
            p.dma("pool", wdn[ws], d_wd[i * 128:(i + 1) * 128], writes=[web[ws]])

        def emit_GU(st, n, fcs=(0, 1)):
            i, e, tq = st
            ws = i % 2
            sl = n % 2
            tq5 = slice(tq * 512, (tq + 1) * 512)
            for fc in fcs:
                bG, _, bGb = pbank("s8g", [0, 1, 2, 3])
                bU, _, bUb = pbank("s8g", [0, 1, 2, 3])

                def fgu(e_, bG=bG, bU=bU, fc=fc):
                    ins = None
                    for kc in range(8):
                        ins = e_.matmul(bG, lhsT=wgu[ws][:, kc, fc * 128:(fc + 1) * 128], rhs=hT[:, kc, tq5],
                                        start=(kc == 0), stop=(kc == 7))
                    for kc in range(8):
                        ins = e_.matmul(bU, lhsT=wgu[ws][:, kc, 256 + fc * 128:256 + (fc + 1) * 128], rhs=hT[:, kc, tq5],
                                        start=(kc == 0), stop=(kc == 7))
                    return ins
                p.op("pe", fgu, reads=[web[ws]] + b_h2[tq * 4:(tq + 1) * 4], writes=[bGb, bUb])
                p.op("act", lambda e_, bG=bG, fc=fc: e_.activation(out=sg[sl][:, fc, :], in_=bG, func=AF.Silu),
                     reads=[bGb], writes=[sgb[sl]])
                p.op("dve", lambda e_, bU=bU, fc=fc: e_.tensor_tensor(out=hid[sl][:, fc, :], in0=sg[sl][:, fc, :], in1=bU,
                                                                      op=ALU.mult), reads=[sgb[sl], bUb], writes=[hidb[sl]])

        def emit_D(st, n, t4s=(0, 1, 2, 3)):
            i, e, tq = st
            ws = i % 2
            sl = n % 2
            for t4 in t4s:
                tt = tq * 4 + t4
                bks = [pbank("s8d", [4, 5, 6, 7]) for _ in range(2)]

                def fd(e_, bks=bks, t4=t4):
                    ins = None
                    for half in range(2):
                        for fc in range(2):
                            ins = e_.matmul(bks[half][0], lhsT=hid[sl][:, fc, t4 * 128:(t4 + 1) * 128],
                                            rhs=wdn[ws][:, fc, half * 512:(half + 1) * 512], start=(fc == 0), stop=(fc == 1))
                    return ins
                p.op("pe", fd, reads=[hidb[sl], web[ws]], writes=[x[2] for x in bks])
                for half in range(2):
                    hs = slice(half * 512, (half + 1) * 512)
                    if e == NE:
                        sc_ = 1.0
                    else:
                        sc_ = Gw[:, tt, e:e + 1]
                    if i == 0:
                        p.op("dve", lambda e_, bks=bks, half=half, hs=hs, sc_=sc_, tt=tt: e_.tensor_scalar_mul(
                            out=acc[:, tt, hs], in0=bks[half][0], scalar1=sc_), reads=[bks[half][2], b_Gw[tt]],
                            writes=[b_acc[tt]])
                    else:
                        p.op("dve", lambda e_, bks=bks, half=half, hs=hs, sc_=sc_, tt=tt: e_.scalar_tensor_tensor(
                            out=acc[:, tt, hs], in0=bks[half][0], scalar=sc_, in1=acc[:, tt, hs], op0=ALU.mult, op1=ALU.add),
                            reads=[bks[half][2], b_Gw[tt]], writes=[b_acc[tt]])

        load(0)
        emit_GU(steps[0], 0)
        for n, st in enumerate(steps):
            if st[2] == 0 and st[0] + 1 < len(exps):
                load(st[0] + 1)
            if n + 1 < len(steps):
                emit_GU(steps[n + 1], n + 1, fcs=(0,))
            emit_D(st, n, t4s=(0, 1))
            if n + 1 < len(steps):
                emit_GU(steps[n + 1], n + 1, fcs=(1,))
            emit_D(st, n, t4s=(2, 3))

        p.barrier()
        xb = [B(), B(), B(), B()]
        ob = [B(), B(), B(), B()]
        for tt in range(16):
            sl = tt % 4
            rows = slice(row0 + tt * 128, row0 + (tt + 1) * 128)
            p.dma("sp", xt3[sl], d_xscr[rows, :], reads=[b_xscr[b][tt]], writes=[xb[sl]])
            p.op("dve", lambda e, sl=sl, tt=tt: e.tensor_tensor(out=ot[sl], in0=acc[:, tt, :], in1=Gtm, op=ALU.mult),
                 reads=[b_acc[tt]], writes=[ob[sl]])
            p.op("dve", lambda e, sl=sl: e.tensor_tensor(out=ot[sl], in0=ot[sl], in1=xt3[sl], op=ALU.add),
                 reads=[xb[sl]], writes=[ob[sl]])
            bo = B()
            p.dma("sp", d_out[rows, :], ot[sl], reads=[ob[sl]], writes=[bo])
            out_bufs.append(bo)
        p.barrier()

    if mode == "mixer":
        xb = [B(), B()]
        for b in range(NB):
            for tt in range(16):
                sl = tt % 2
                rows = slice(b * S + tt * 128, b * S + (tt + 1) * 128)
                p.dma("sp", xt3[sl], d_xscr[rows, :], reads=[b_xscr[b][tt]], writes=[xb[sl]])
                bo = B()
                p.dma("sp", d_out[rows, :], xt3[sl], reads=[xb[sl]], writes=[bo])
                out_bufs.append(bo)
    p.op("sp", None, reads=out_bufs)

    p.emit(nc)
    return nc


def _rope_tables():
    pos = np.arange(S)
    row = (pos // 64).astype(np.float32)
    col = (pos % 64).astype(np.float32)
    inv = (10000.0 ** (-np.arange(0, 32, 2, dtype=np.float32) / 32)).astype(np.float32)
    ar = row[:, None] * inv[None, :]
    ac = col[:, None] * inv[None, :]
    cr, sr, cc_, sc_ = np.cos(ar), np.sin(ar), np.cos(ac), np.sin(ac)
    CC = np.concatenate([cr, cr, cc_, cc_], axis=1)
    SS = np.concatenate([-sr, sr, -sc_, sc_], axis=1)
    t = np.concatenate([CC, SS], axis=1).astype(np.float32)
    return np.ascontiguousarray(t.reshape(16, 128, 128).transpose(1, 0, 2))


def _prep_shared(inp):
    f = lambda a: np.asarray(a, dtype=np.float32)
    kc = lambda w: np.ascontiguousarray(w.reshape(8, 128, -1).transpose(1, 0, 2))
    sh = {}
    wada = f(inp["w_ada"])[0]
    sh["wada"] = np.ascontiguousarray(wada.reshape(8, 128, 6, 1024).transpose(2, 1, 0, 3)).reshape(6 * 128, 8, 1024)
    sh["bada"] = np.ascontiguousarray(f(inp["b_ada"])[0].reshape(48, 128).T)
    sh["gmix"] = np.ascontiguousarray(f(inp["g_norm_mix"])[0].reshape(8, 128).T)
    sh["gffn"] = np.ascontiguousarray(f(inp["g_norm_ffn"])[0].reshape(8, 128).T)
    sh["win"] = kc(f(inp["w_in"])[0])
    g10 = np.concatenate([np.tile(f(inp["q_norm_g"])[0], 8), np.tile(f(inp["k_norm_g"])[0], 2)])
    sh["g10"] = np.ascontiguousarray(np.broadcast_to(g10[None, :], (128, 640)))
    sh["convw"] = np.ascontiguousarray(f(inp["conv_w"])[0].reshape(3, 4, 128).transpose(2, 1, 0))
    sh["wao"] = np.ascontiguousarray(f(inp["w_attn_o"])[0].reshape(8, 64, 1024).transpose(1, 0, 2))
    sh["wco"] = np.ascontiguousarray(f(inp["w_conv_o"])[0].reshape(4, 128, 1024).transpose(1, 0, 2))
    sh["wout"] = kc(f(inp["w_out"])[0])
    sh["wr"] = kc(f(inp["w_router"])[0])
    sh["rbias"] = np.ascontiguousarray(np.broadcast_to(f(inp["router_bias"])[0][None, :], (128, 256)))
    wg = np.concatenate([f(inp["w_gate_e"])[0], f(inp["w_gate_s"])[0][None]], axis=0)
    wu = np.concatenate([f(inp["w_up_e"])[0], f(inp["w_up_s"])[0][None]], axis=0)
    wgu = np.concatenate([wg, wu], axis=2)
    del wg, wu
    sh["wgu"] = np.ascontiguousarray(wgu.reshape(NE + 1, 8, 128, 512).transpose(0, 2, 1, 3)).reshape((NE + 1) * 128, 8, 512)
    del wgu
    wd = np.concatenate([f(inp["w_down_e"])[0], f(inp["w_down_s"])[0][None]], axis=0)
    sh["wd"] = np.ascontiguousarray(wd.reshape(NE + 1, 2, 128, 1024).transpose(0, 2, 1, 3)).reshape((NE + 1) * 128, 2, 1024)
    del wd
    sh["ccss"] = _rope_tables()
    sh["ident"] = np.eye(128, dtype=np.float32)
    return sh


def kernel(**inputs):
    x = np.asarray(inputs["x"], dtype=np.float32)
    c = np.asarray(inputs["c"], dtype=np.float32)
    sh = _prep_shared(inputs)
    in_maps = []
    for core in range(NCORES):
        m = dict(sh)
        m["xs"] = np.ascontiguousarray(x[NB * core:NB * (core + 1)].reshape(NB * S, D))
        cT = c[NB * core:NB * (core + 1)].T
        m["cT"] = np.ascontiguousarray(cT.reshape(8, 128, NB).transpose(1, 0, 2))
        in_maps.append(m)
    nc = build_program("full")
    res = run_bass_kernel_spmd(nc, in_maps, core_ids=list(range(NCORES)))
    out = np.concatenate([res.results[i]["out"].reshape(NB, S, D) for i in range(NCORES)], axis=0)
    return out.astype(np.float32)
```
